# Optimizing a Trainium2 kernel written in Bass

```python
import math
import jax, jax.numpy as jnp
from jax import lax
import numpy as np

D_MODEL = 2048
BATCH = 2
SEQ = 4096
DEPTH = 1

EPS = 1e-6
GLA_HEADS = 8
GLA_DV = D_MODEL // 2 // GLA_HEADS
GLA_DK = GLA_DV // 2
GLA_GATE_RANK = 16
GLA_GATE_NORMALIZER = 16.0
GLA_CHUNK = 64
MOBA_HEADS = 8
MOBA_HEAD_DIM = D_MODEL // 2 // MOBA_HEADS
MOBA_BLOCK = 256
MOBA_TOPK = 3
MOBA_QCHUNK = 32
ROPE_THETA = 500000.0
ROPE_DIMS = MOBA_HEAD_DIM // 4
PEER_HEADS = 8
PEER_N_KEYS = 128
PEER_N_EXPERTS = PEER_N_KEYS * PEER_N_KEYS
PEER_QUERY_DIM = 256
PEER_TOPK = 16
PEER_TOKEN_CHUNK = 128
IN_SPLITS = (GLA_HEADS * GLA_DK, GLA_HEADS * GLA_DK, GLA_HEADS * GLA_DV, GLA_HEADS * GLA_DV, GLA_GATE_RANK,
             MOBA_HEADS * MOBA_HEAD_DIM, MOBA_HEADS * MOBA_HEAD_DIM, MOBA_HEADS * MOBA_HEAD_DIM)
IN_WIDTH = sum(IN_SPLITS)
MIX_WIDTH = GLA_HEADS * GLA_DV + MOBA_HEADS * MOBA_HEAD_DIM

kernel_name = "hybrid_gla_moba_peer_adaln"


def rms_norm(x, g):
    xf = x.astype(jnp.float32)
    y = xf * lax.rsqrt(jnp.mean(xf * xf, axis=-1, keepdims=True) + EPS)
    return (y * g.astype(jnp.float32)).astype(x.dtype)


def modulate(h, shift, scale):
    return h * (1.0 + scale[:, None, :]) + shift[:, None, :]


def partial_rotary(x, positions):
    half = ROPE_DIMS // 2
    inv_freq = jnp.power(jnp.float32(ROPE_THETA), -jnp.arange(half, dtype=jnp.float32) * (2.0 / ROPE_DIMS))
    ang = positions.astype(jnp.float32)[:, None, :, None] * inv_freq
    cos, sin = jnp.cos(ang), jnp.sin(ang)
    xr = x[..., :ROPE_DIMS].astype(jnp.float32)
    x1, x2 = xr[..., :half], xr[..., half:]
    rot = jnp.concatenate([x1 * cos - x2 * sin, x2 * cos + x1 * sin], axis=-1)
    return jnp.concatenate([rot.astype(x.dtype), x[..., ROPE_DIMS:]], axis=-1)


def gla_attention(q, k, v, log_a):
    B, H, S, DK = q.shape
    DV = v.shape[-1]
    L = GLA_CHUNK
    n = S // L
    f32 = jnp.float32
    rs = lambda t: t.astype(f32).reshape(B, H, n, L, t.shape[-1])
    q = rs(q) * (DK ** -0.5)
    k, v, log_a = rs(k), rs(v), rs(log_a)
    b = jnp.cumsum(log_a, axis=3)
    q_dec = q * jnp.exp(b)
    k_dec = k * jnp.exp(-b)
    causal = jnp.tril(jnp.ones((L, L), dtype=bool))
    attn = jnp.where(causal, jnp.einsum('bhnld,bhnmd->bhnlm', q_dec, k_dec), 0.0)
    o_intra = jnp.einsum('bhnlm,bhnmv->bhnlv', attn, v)
    b_last = b[:, :, :, -1:, :]
    chunk_update = jnp.einsum('bhnld,bhnlv->bhndv', k * jnp.exp(b_last - b), v)
    chunk_decay = jnp.exp(b_last[:, :, :, 0, :])

    def step(state, inp):
        dec, upd = inp
        return dec[..., None] * state + upd, state

    _, states = lax.scan(step, jnp.zeros((B, H, DK, DV), f32),
                         (jnp.moveaxis(chunk_decay, 2, 0), jnp.moveaxis(chunk_update, 2, 0)))
    states = jnp.moveaxis(states, 0, 2)
    o_inter = jnp.einsum('bhnld,bhndv->bhnlv', q_dec, states)
    return (o_intra + o_inter).reshape(B, H, S, DV)


def moba_attention(q, k, v):
    B, H, S, Dh = q.shape
    P = MOBA_BLOCK
    S_pad = -(-S // P) * P
    pad = S_pad - S
    if pad:
        padw = ((0, 0), (0, 0), (0, pad), (0, 0))
        q, k, v = jnp.pad(q, padw), jnp.pad(k, padw), jnp.pad(v, padw)
    nb = S_pad // P
    kb = k.reshape(B, H, nb, P, Dh)
    vb = v.reshape(B, H, nb, P, Dh)
    k_mean = jnp.mean(kb.astype(jnp.float32), axis=3)
    gate = jnp.einsum('bhsd,bhnd->bhsn', q.astype(jnp.float32), k_mean)
    q_block = jnp.arange(S_pad) // P
    past = jnp.arange(nb)[None, :] < q_block[:, None]
    gate = jnp.where(past, gate, -jnp.inf)
    n_sel = min(MOBA_TOPK, nb)
    _, sel = lax.top_k(gate, n_sel)
    sel_valid = sel < q_block[:, None]
    scale = Dh ** -0.5
    QC = MOBA_QCHUNK
    nq = S_pad // QC
    to_chunks = lambda t: jnp.moveaxis(t.reshape(B, H, nq, QC, t.shape[-1]), 2, 0)
    b_idx = jnp.arange(B)[:, None, None, None]
    h_idx = jnp.arange(H)[None, :, None, None]

    def chunk(args):
        ci, q_c, sel_c, valid_c = args
        blk = (ci * QC) // P
        k_sel = kb[b_idx, h_idx, sel_c]
        v_sel = vb[b_idx, h_idx, sel_c]
        k_own = lax.dynamic_index_in_dim(kb, blk, axis=2, keepdims=False)
        v_own = lax.dynamic_index_in_dim(vb, blk, axis=2, keepdims=False)
        s_sel = jnp.einsum('bhqd,bhqnpd->bhqnp', q_c, k_sel, preferred_element_type=jnp.float32) * scale
        s_sel = jnp.where(valid_c[..., None], s_sel, -jnp.inf).reshape(B, H, QC, n_sel * P)
        s_own = jnp.einsum('bhqd,bhpd->bhqp', q_c, k_own, preferred_element_type=jnp.float32) * scale
        q_pos = ci * QC + jnp.arange(QC)
        k_pos = blk * P + jnp.arange(P)
        s_own = jnp.where(k_pos[None, :] <= q_pos[:, None], s_own, -jnp.inf)
        p = jax.nn.softmax(jnp.concatenate([s_sel, s_own], axis=-1), axis=-1)
        p_sel = p[..., :n_sel * P].reshape(B, H, QC, n_sel, P).astype(v.dtype)
        p_own = p[..., n_sel * P:].astype(v.dtype)
        return (jnp.einsum('bhqnp,bhqnpd->bhqd', p_sel, v_sel)
                + jnp.einsum('bhqp,bhpd->bhqd', p_own, v_own))

    out = lax.map(chunk, (jnp.arange(nq), to_chunks(q), to_chunks(sel), to_chunks(sel_valid)))
    out = jnp.moveaxis(out, 0, 2).reshape(B, H, S_pad, Dh)
    return out[:, :, :S]


def peer_ffn(h, w_q, sub_keys, u, v):
    B, S, D = h.shape
    T = B * S
    K = PEER_TOPK
    hf = h.reshape(T, D)
    q = (hf @ w_q).reshape(T, PEER_HEADS, 2, PEER_QUERY_DIM // 2)
    s = jnp.einsum('thcd,hckd->thck', q, sub_keys, preferred_element_type=jnp.float32)
    s1, i1 = lax.top_k(s[:, :, 0], K)
    s2, i2 = lax.top_k(s[:, :, 1], K)
    cand_s = (s1[..., :, None] + s2[..., None, :]).reshape(T, PEER_HEADS, K * K)
    cand_i = (i1[..., :, None] * PEER_N_KEYS + i2[..., None, :]).reshape(T, PEER_HEADS, K * K)
    top_s, pos = lax.top_k(cand_s, K)
    expert = jnp.take_along_axis(cand_i, pos, axis=-1)
    gates = jax.nn.softmax(top_s, axis=-1)
    TC = PEER_TOKEN_CHUNK
    nc = T // TC

    def chunk(args):
        h_c, e_c, g_c = args
        act = jax.nn.gelu(jnp.einsum('td,thkd->thk', h_c, u[e_c], preferred_element_type=jnp.float32),
                          approximate=False)
        w = (g_c * act).astype(h.dtype)
        return jnp.einsum('thk,thkd->td', w, v[e_c])

    out = lax.map(chunk, (hf.reshape(nc, TC, D), expert.reshape(nc, TC, PEER_HEADS, K),
                          gates.reshape(nc, TC, PEER_HEADS, K)))
    return out.reshape(B, S, D)


def setup_inputs(seed: int = 0) -> dict:
    key = jax.random.key(seed)
    ks = jax.random.split(key, 16)
    D = D_MODEL
    nrm = lambda k, shape, s: jax.random.normal(k, shape, jnp.float32) * s
    return {
        "x": nrm(ks[0], (BATCH, SEQ, D), 1.0),
        "c": nrm(ks[1], (BATCH, D), 1.0),
        "positions": jnp.broadcast_to(jnp.arange(SEQ, dtype=jnp.int32)[None, :], (BATCH, SEQ)),
        "w_ada": nrm(ks[2], (D, 6 * D), 0.5 * D ** -0.5),
        "b_ada": nrm(ks[3], (6 * D,), 0.01),
        "norm1_g": 1.0 + nrm(ks[4], (D,), 0.02),
        "w_in": nrm(ks[5], (D, IN_WIDTH), D ** -0.5),
        "gla_w_a2": nrm(ks[6], (GLA_GATE_RANK, GLA_HEADS * GLA_DK), GLA_GATE_RANK ** -0.5),
        "gla_b_a": nrm(ks[7], (GLA_HEADS * GLA_DK,), 0.1),
        "gla_norm_g": 1.0 + nrm(ks[8], (GLA_DV,), 0.02),
        "moba_q_norm_g": 1.0 + nrm(ks[9], (MOBA_HEAD_DIM,), 0.02),
        "moba_k_norm_g": 1.0 + nrm(ks[10], (MOBA_HEAD_DIM,), 0.02),
        "w_out": nrm(ks[11], (MIX_WIDTH, D), MIX_WIDTH ** -0.5),
        "norm2_g": 1.0 + nrm(ks[12], (D,), 0.02),
        "peer_w_q": nrm(ks[13], (D, PEER_HEADS * PEER_QUERY_DIM), D ** -0.5),
        "peer_sub_keys": nrm(ks[14], (PEER_HEADS, 2, PEER_N_KEYS, PEER_QUERY_DIM // 2), (PEER_QUERY_DIM // 2) ** -0.5),
        "peer_u": nrm(jax.random.fold_in(ks[15], 0), (PEER_N_EXPERTS, D), D ** -0.5),
        "peer_v": nrm(jax.random.fold_in(ks[15], 1), (PEER_N_EXPERTS, D), 1.0),
    }


def reference(x, c, positions, w_ada, b_ada, norm1_g, w_in, gla_w_a2, gla_b_a, gla_norm_g,
              moba_q_norm_g, moba_k_norm_g, w_out, norm2_g, peer_w_q, peer_sub_keys, peer_u, peer_v):
    B, S, D = x.shape
    for _ in range(DEPTH):
        mod = jax.nn.silu(c) @ w_ada + b_ada
        shift1, scale1, gate1, shift2, scale2, gate2 = jnp.split(mod, 6, axis=-1)

        h = modulate(rms_norm(x, norm1_g), shift1, scale1)
        proj = h @ w_in
        offs = np.cumsum(IN_SPLITS)[:-1].tolist()
        gq, gk, gv, gg, ga, mq, mk, mv = jnp.split(proj, offs, axis=-1)

        heads = lambda t, nh: jnp.transpose(t.reshape(B, S, nh, -1), (0, 2, 1, 3))
        log_a = jax.nn.log_sigmoid((ga @ gla_w_a2 + gla_b_a).astype(jnp.float32)) / GLA_GATE_NORMALIZER
        o_gla = gla_attention(heads(gq, GLA_HEADS), heads(gk, GLA_HEADS), heads(gv, GLA_HEADS),
                              heads(log_a, GLA_HEADS)).astype(x.dtype)
        o_gla = rms_norm(jnp.transpose(o_gla, (0, 2, 1, 3)), gla_norm_g)
        o_gla = (o_gla * jax.nn.silu(gg.reshape(B, S, GLA_HEADS, GLA_DV))).reshape(B, S, GLA_HEADS * GLA_DV)

        qm = rms_norm(mq.reshape(B, S, MOBA_HEADS, MOBA_HEAD_DIM), moba_q_norm_g)
        km = rms_norm(mk.reshape(B, S, MOBA_HEADS, MOBA_HEAD_DIM), moba_k_norm_g)
        qm = partial_rotary(jnp.transpose(qm, (0, 2, 1, 3)), positions)
        km = partial_rotary(jnp.transpose(km, (0, 2, 1, 3)), positions)
        o_moba = moba_attention(qm, km, heads(mv, MOBA_HEADS))
        o_moba = jnp.transpose(o_moba, (0, 2, 1, 3)).reshape(B, S, MOBA_HEADS * MOBA_HEAD_DIM)

        mix = jnp.concatenate([o_gla, o_moba], axis=-1) @ w_out
        x = x + gate1[:, None, :] * mix

        h2 = modulate(rms_norm(x, norm2_g), shift2, scale2)
        x = x + gate2[:, None, :] * peer_ffn(h2, peer_w_q, peer_sub_keys, peer_u, peer_v)
    return x
```

```python
import contextlib
import numpy as np
import ml_dtypes
import concourse.bass as bass
import concourse.mybir as mybir
from concourse.bass_utils import run_bass_kernel_spmd

F32 = mybir.dt.float32
BF16 = mybir.dt.bfloat16
I32 = mybir.dt.int32
AF = mybir.ActivationFunctionType
ALU = mybir.AluOpType
AX = mybir.AxisListType

D = 2048
KC = 16
SEQ = 4096
NTOK = 8192
NT = 32
EPS = 1e-6
NEG = -30000.0


class Buf:
    __slots__ = ("name", "writer", "readers", "dsem", "dcnt", "excl")

    def __init__(self, name, excl=False):
        self.name = name
        self.excl = excl
        self.writer = None
        self.readers = []
        self.dsem = None
        self.dcnt = 0


class Eng:
    def __init__(self, name, h, is_pe=False):
        self.name = name
        self.h = h
        self.sem = None
        self.cnt = 0
        self.total = 0
        self.is_pe = is_pe
        self.gen = 0
        self.tokmap = {}
        self.seen_idx = -1


class K:
    ROLL = 30000

    def __init__(self, nc, needed=None):
        self.nc = nc
        self.record = needed is None
        self.needed = set() if needed is None else needed
        self.pe = Eng("pe", nc.tensor, True)
        self.act = Eng("act", nc.scalar)
        self.dve = Eng("dve", nc.vector)
        self.pool = Eng("pool", nc.gpsimd)
        self.sp = Eng("sp", nc.sync)
        self.engs = [self.pe, self.act, self.dve, self.pool, self.sp]
        self.by_name = {e.name: e for e in self.engs}
        for e in self.engs:
            e.sem = nc.alloc_semaphore(f"s_{e.name}_0")
            e.seen = {}
        self.dbufs = []
        self.n = 0

    def buf(self, name):
        return Buf(name)

    def bufs(self, name, n):
        return [Buf(f"{name}{i}") for i in range(n)]

    def _wait(self, eng, tok):
        if tok[0] == "E":
            _, src, idx = tok
            if src == eng.name and eng.is_pe:
                return
            if eng.seen.get(src, -1) >= idx:
                return
            eng.seen[src] = idx
            if self.record:
                self.needed.add((src, idx))
                return
            sem, val = self.by_name[src].tokmap[idx]
            eng.h.wait_ge(sem, val)
        else:
            _, sem, val = tok
            key = ("d", sem.num)
            if eng.seen.get(key, 0) >= val:
                return
            eng.seen[key] = val
            if not self.record:
                eng.h.wait_ge(sem, val)

    def _deps(self, eng, reads, writes):
        for b in reads:
            if b.writer is not None:
                self._wait(eng, b.writer)
            if b.excl:
                for t in b.readers:
                    if not (t[0] == "E" and t[1] == eng.name):
                        self._wait(eng, t)
        for b in writes:
            if b.writer is not None:
                self._wait(eng, b.writer)
            for t in b.readers:
                self._wait(eng, t)

    def _retire(self, tok, reads, writes):
        for b in writes:
            b.writer = tok
            b.readers = []
        for b in reads:
            if b not in writes:
                b.readers.append(tok)
                if len(b.readers) > 40:
                    b.readers = b.readers[-40:]
        self.n += 1

    def op(self, eng, fn, reads=(), writes=()):
        self._deps(eng, reads, writes)
        idx = eng.total
        eng.total += 1
        ins = fn(eng.h)
        if (not self.record) and (eng.name, idx) in self.needed:
            if eng.cnt >= self.ROLL:
                eng.gen += 1
                eng.sem = self.nc.alloc_semaphore(f"s_{eng.name}_{eng.gen}")
                eng.cnt = 0
            eng.cnt += 1
            ins.then_inc(eng.sem, 1)
            eng.tokmap[idx] = (eng.sem, eng.cnt)
        self._retire(("E", eng.name, idx), reads, writes)
        return ins

    def dma(self, eng, out, in_, reads=(), writes=(), sembuf=None, inc=16, fn=None, cont=False, **kw):
        if not cont:
            self._deps(eng, reads, writes)
        sb = sembuf or (writes[0] if writes else reads[0])
        if sb.dsem is None:
            sb.dsem = self.nc.alloc_semaphore(f"d{len(self.dbufs)}_" + sb.name)
            self.dbufs.append(sb)
        if fn is None:
            ins = eng.h.dma_start(out=out, in_=in_, **kw)
        else:
            ins = fn(eng.h)
        ins.then_inc(sb.dsem, inc)
        sb.dcnt += inc
        self._retire(("D", sb.dsem, sb.dcnt), reads, writes)
        return ins

    def barrier(self):
        toks = []
        for f in self.engs:
            if f.total > 0:
                toks.append(("E", f.name, f.total - 1))
        for b in self.dbufs:
            if b.dcnt > 0:
                toks.append(("D", b.dsem, b.dcnt))
        for e in self.engs:
            for t in toks:
                self._wait(e, t)


def _consts():
    c = {}
    ident = np.eye(128, dtype=np.float32)
    m = np.arange(128)[:, None]
    l = np.arange(128)[None, :]
    triU = (m <= l).astype(np.float32)
    triL = (m > l).astype(np.float32)
    f32 = np.zeros((128, 1536), np.float32)
    f32[:, 0:128] = ident
    f32[:, 128:256] = triU
    f32[:, 256:384] = triU * (-1.0 / 16.0)
    f32[:, 384:512] = triL * (-1.0 / 16.0)
    f32[:, 512:640] = 1.0
    nb = np.arange(16)[:, None]
    n = np.arange(16)[None, :]
    f32[:, 640:896] = np.broadcast_to(np.where(n < nb, 0.0, -1e30).reshape(1, 256), (128, 256))
    f32[:, 896:1152] = np.broadcast_to((n < nb).astype(np.float32).reshape(1, 256), (128, 256))
    half = 16
    inv = np.power(np.float32(500000.0), -np.arange(half, dtype=np.float32) * np.float32(2.0 / 32)).astype(np.float32)
    f32[:, 1152:1168] = inv[None, :]
    f32[:, 1168:1169] = -1.0 / 16.0
    f32[:, 1169:1170] = EPS
    c["cf"] = f32
    b16 = np.zeros((128, 2432), np.float32)
    b16[:, 0:128] = ident
    b16[:, 128:256] = 1.0
    b16[:, 256:384] = triU
    ind = np.zeros((128, 16, 128), np.float32)
    for q in range(16):
        ind[q, q, :] = 1.0
    b16[:, 384:2432] = ind.reshape(128, 2048)
    c["cb"] = b16.astype(ml_dtypes.bfloat16)
    return c


def build(stage=9, dbg=False):
    k0 = _build(stage, dbg, None)
    return _build(stage, dbg, k0.needed).nc


def _build(stage, dbg, needed):
    nc = bass.Bass("TRN2", target_bir_lowering=False)
    k = K(nc, needed)
    pe, act, dve, pool, sp = k.pe, k.act, k.dve, k.pool, k.sp

    def din(name, shape, dt=F32):
        return nc.dram_tensor(name, list(shape), dt, kind="ExternalInput")

    x_all = din("x_all", [NTOK, D])
    x_own = din("x_own", [1024, D])
    c3 = din("c3", [128, KC * 3])
    w_ada = din("w_ada", [D, 6 * D])
    b_ada_fm = din("b_ada_fm", [128, 96])
    g12_fm = din("g12_fm", [128, 32])
    w_in_c = din("w_in_c", [D, 784])
    wa2 = din("wa2", [17, 64])
    vecbc = din("vecbc", [128, 384])
    pos_t = din("pos_t", [128, 64], I32)
    if stage >= 2:
        w_out_p = din("w_out_p", [D, D])
    if stage >= 3:
        w_q = din("w_q", [D, D])
        skT = din("skT", [128, 16 * 128])
        uT = din("uT", [D, 16384])
        vv = din("vv", [16384, D])
    cf_d = din("cf", [128, 1536])
    cb_d = din("cb", [128, 2432], BF16)
    y = nc.dram_tensor("y", [1024, D], F32, kind="ExternalOutput")
    ib = nc.dram_tensor("ib", [256, NTOK], BF16, kind="Internal")
    ob = nc.dram_tensor("ob", [2048, NTOK], BF16, kind="Internal")
    x1d = nc.dram_tensor("x1d", [1024, D], F32, kind="Internal")
    if dbg:
        d_mod = nc.dram_tensor("d_mod", [128, 288], F32, kind="ExternalOutput")
        d_ib = nc.dram_tensor("d_ib", [256, NTOK], BF16, kind="ExternalOutput")
        d_x1 = nc.dram_tensor("d_x1", [1024, D], F32, kind="ExternalOutput")
        d_dbg = nc.dram_tensor("d_dbg", [128, 4096], F32, kind="ExternalOutput")
        dbgt = nc.alloc_sbuf_tensor("dbgt", [128, 4096 if stage == 1 else 16], F32)
        b_dbgt = Buf("dbgt")
        nc.vector.memset(dbgt[:], 0.0)

    def ddump(c0, src, reads, p=128):
        if not dbg or stage != 1:
            return
        w = src.shape[-1]
        k.op(act, lambda e: e.activation(out=dbgt[0:p, c0:c0 + w], in_=src, func=AF.Copy), reads=list(reads), writes=[b_dbgt])

    PS = [nc.alloc_psum_tensor(f"ps{i}", [128, 512], F32) for i in range(8)]
    PB_ = [Buf(f"ps{i}", excl=True) for i in range(8)]

    def sb(name, shape, dt=F32):
        return nc.alloc_sbuf_tensor(name, list(shape), dt)

    cf = sb("cf_s", [128, 1536]); b_cf = k.buf("cf")
    cb = sb("cb_s", [128, 2432], BF16); b_cb = k.buf("cb")
    ident = cf[:, 0:128]
    triU = cf[:, 128:256]
    triUs = cf[:, 256:384]
    triLs = cf[:, 384:512]
    onesf = cf[:, 512:640]
    PBt = cf[:, 640:896]
    PMt = cf[:, 896:1152]
    invf = cf[:, 1152:1168]
    negs = cf[:, 1168:1169]
    epsc = cf[:, 1169:1170]
    identb = cb[:, 0:128]
    onesb = cb[:, 128:256]
    triUb = cb[:, 256:384]
    indb = cb[:, 384:2432].rearrange("p (a m) -> p a m", a=16)

    mod = sb("mod", [128, 96, 3]); b_mod = k.buf("mod")
    A1 = sb("A1", [128, KC, 2]); b_A1 = k.buf("A1")
    B1p = sb("B1p", [128, KC, 2], BF16); b_B1p = k.buf("B1p")
    g12 = sb("g12", [128, 32]); b_g12 = k.buf("g12")

    k.dma(sp, cf[:], cf_d.ap(), writes=[b_cf])
    k.dma(sp, cb[:], cb_d.ap(), writes=[b_cb])
    k.dma(sp, g12[:], g12_fm.ap(), writes=[b_g12])

    with contextlib.ExitStack() as es:
        c3s = es.enter_context(nc.sbuf_tensor("c3s", [128, KC, 3], F32)); b_c3 = k.buf("c3s")
        bada = es.enter_context(nc.sbuf_tensor("bada", [128, 96], F32)); b_bada = k.buf("bada")
        wa = [es.enter_context(nc.sbuf_tensor(f"wada{i}", [128, KC, 512], BF16)) for i in range(4)]
        c3b = es.enter_context(nc.sbuf_tensor("c3b", [128, KC, 3], BF16)); b_c3b = k.buf("c3b")
        b_wa = k.bufs("wa", 4)
        vt = es.enter_context(nc.sbuf_tensor("vt", [128, 64], F32)); b_vt = k.buf("vt")
        k.dma(sp, c3s[:], c3.ap().rearrange("p (k b) -> p k b", b=3), writes=[b_c3])
        k.dma(sp, bada[:], b_ada_fm.ap(), writes=[b_bada])
        k.op(act, lambda e: e.activation(out=c3b[:], in_=c3s[:], func=AF.Silu), reads=[b_c3], writes=[b_c3b])
        pm = PS[0]
        b_pm = PB_[0]
        wsrc = w_ada.ap().rearrange("(k p) c -> p k c", p=128)
        import os as _os
        for blk in range(0 if (_os.environ.get('DEV_SKIP01') or _os.environ.get('DEV_SKIP0')) else 24):
            s = blk % 4
            for hh in range(2):
                k.dma(pool, wa[s][:, hh * 8:(hh + 1) * 8, :], wsrc[:, hh * 8:(hh + 1) * 8, blk * 512:(blk + 1) * 512],
                      writes=[b_wa[s]], cont=(hh > 0))
            for jj in range(4):
                j = blk * 4 + jj
                for kk in range(KC):
                    k.op(pe, lambda e, j=j, jj=jj, kk=kk, s=s: e.matmul(
                        pm[:, j * 3:(j + 1) * 3], lhsT=wa[s][:, kk, jj * 128:(jj + 1) * 128], rhs=c3b[:, kk, :],
                        start=(kk == 0), stop=(kk == KC - 1)),
                        reads=[b_wa[s], b_c3b], writes=[b_pm])
        k.op(dve, lambda e: e.tensor_tensor(mod[:], pm[:, 0:288].rearrange("p (j b) -> p j b", b=3),
                                            bada[:].unsqueeze(2).to_broadcast([128, 96, 3]), ALU.add),
             reads=[b_pm, b_bada], writes=[b_mod])
        k.op(dve, lambda e: e.scalar_tensor_tensor(out=A1[:], in0=mod[:, 16:32, 0:2], scalar=1.0,
                                                   in1=g12[:, 0:16].unsqueeze(2).to_broadcast([128, KC, 2]),
                                                   op0=ALU.add, op1=ALU.mult),
             reads=[b_mod, b_g12], writes=[b_A1])
        k.op(dve, lambda e: e.reciprocal(vt[:, 0:32].rearrange("p (k b) -> p k b", b=2), A1[:]), reads=[b_A1], writes=[b_vt])
        k.op(dve, lambda e: e.tensor_tensor(B1p[:], vt[:, 0:32].rearrange("p (k b) -> p k b", b=2), mod[:, 0:16, 0:2], ALU.mult),
             reads=[b_vt, b_mod], writes=[b_B1p])
        if dbg:
            k.dma(sp, d_mod.ap(), mod[:].rearrange("p j b -> p (j b)"), reads=[b_mod], sembuf=k.buf("dmod"))
        k.barrier()
    if stage <= 0:
        k.barrier()
        return k


    bcn = [0]

    def build_bcast(dsts):
        bcn[0] += 1
        with contextlib.ExitStack() as es2:
            dg = [es2.enter_context(nc.sbuf_tensor(f"dg{i}_{bcn[0]}", [128, 128], F32)) for i in range(2)]
            b_dg = k.bufs("dg", 2)
            vt = es2.enter_context(nc.sbuf_tensor(f"vt2_{bcn[0]}", [128, 16], F32)); b_vt = k.buf("vt2")
            k.op(dve, lambda e: e.scalar_tensor_tensor(out=vt[:], in0=mod[:, 64:80, 2], scalar=1.0, in1=g12[:, 16:32],
                                                       op0=ALU.add, op1=ALU.mult), reads=[b_mod, b_g12], writes=[b_vt])
            srcm = {"gate1": mod[:, 32:48, 2], "A2": vt[:], "B2": mod[:, 48:64, 2], "gate2": mod[:, 80:96, 2]}
            for (kind, dst, bdst) in dsts:
                src = srcm[kind]
                for kk in range(KC):
                    s_ = kk % 2
                    k.op(dve, lambda e, kk=kk, s_=s_, src=src: e.tensor_scalar(dg[s_][:], ident, src[:, kk:kk + 1], None, ALU.mult),
                         reads=[b_cf, b_mod, b_vt], writes=[b_dg[s_]])
                    bank = 1 + (kk // 4) % 2
                    k.op(pe, lambda e, kk=kk, s_=s_, bank=bank: e.matmul(PS[bank][:, (kk % 4) * 128:(kk % 4 + 1) * 128], lhsT=onesf,
                                                                          rhs=dg[s_][:], start=True, stop=True),
                         reads=[b_cf, b_dg[s_]], writes=[PB_[bank]])
                    if kk % 4 == 3:
                        k.op(act, lambda e, kk=kk, bank=bank, dst=dst: e.activation(
                            out=dst[:, (kk // 4) * 512:(kk // 4 + 1) * 512], in_=PS[bank][:], func=AF.Copy),
                            reads=[PB_[bank]], writes=[bdst])
            k.barrier()

    with contextlib.ExitStack() as es:
        def T(name, shape, dt=F32):
            return es.enter_context(nc.sbuf_tensor(name, list(shape), dt))
        Wst = T("Wst", [128, KC, 784]); b_Wst = k.buf("Wst")
        Wb = T("Wb", [128, KC, 784], BF16); b_Wb = k.buf("Wb")
        brow = T("brow", [1, 784], BF16); b_brow = k.buf("brow")
        wa2s = T("wa2s", [17, 64]); b_wa2 = k.buf("wa2s")
        vbc = T("vbc", [128, 384]); b_vbc = k.buf("vbc")
        posi = T("posi", [128, 64], I32); b_posi = k.buf("posi")
        ang = T("ang", [128, 64, 16]); b_ang = k.buf("ang")
        cosT = T("cosT", [128, 64, 16]); sinT = T("sinT", [128, 64, 16]); b_cs = k.buf("cs")
        NX = 3
        xs = [T(f"xs{i}", [128, D]) for i in range(NX)]; b_xs = k.bufs("xs", NX)
        st = [T(f"st{i}", [128, 8]) for i in range(NX)]; b_st = k.bufs("st", NX)
        hb = [T(f"hb{i}", [128, D], BF16) for i in range(2)]; b_hb = k.bufs("hb", 2)
        hT = [T(f"hT{i}", [128, KC, 128], BF16) for i in range(2)]; b_hT = k.bufs("hT", 2)
        KT = T("KT", [128, SEQ], BF16); b_KT = k.bufs("KT", NT)
        Vt = T("Vt", [128, NT, 132], BF16); b_V = k.bufs("V", NT)
        kmT = T("kmT", [128, 16], BF16); b_km = k.buf("kmT")
        Sf = T("Sf", [64, 128]); b_Sf = k.buf("Sf")
        Sb = [T(f"Sb{i}", [64, 128], BF16) for i in range(2)]; b_Sb = k.bufs("Sb", 2)
        gaA = T("gaA", [17, 128]); b_gaA = k.buf("gaA")
        gas = T("gas", [128, 16]); b_gas = k.buf("gas")
        l1 = T("l1", [128, 64]); b_l1 = k.buf("l1")
        Eb = T("Eb", [128, 192]); b_Eb = k.buf("Eb")
        dec = T("dec", [64, 1]); b_dec = k.buf("dec")
        qkd = T("qkd", [128, 192], BF16); b_qkd = k.buf("qkd")
        qkT = T("qkT", [64, 256], BF16); b_qkT = k.buf("qkT")
        vbf = T("vbf", [128, 128], BF16); b_vbf = k.buf("vbf")
        atT = T("atT", [128, 128], BF16); b_atT = k.buf("atT")
        sg = T("sg", [128, 128]); b_sg = k.buf("sg")
        otmp = T("otmp", [128, 128]); b_otmp = k.buf("otmp")
        ocat = T("ocat", [128, 256], BF16); b_ocat = k.buf("ocat")
        oTs = [T(f"oTs{i}", [128, 2, 512], BF16) for i in range(2)]; b_oTs = k.bufs("oTs", 2)
        qkn = T("qkn", [128, 2, 128]); b_qkn = k.buf("qkn")
        rt = T("rt", [128, 6, 2, 16]); b_rt = k.buf("rt")
        qkb = T("qkb", [128, 2, 128], BF16); b_qkb = k.buf("qkb")
        qTc = T("qTc", [128, 128], BF16); b_qTc = k.buf("qTc")
        gsb = T("gsb", [128, 16]); b_gsb = k.buf("gsb")
        m8 = T("m8", [128, 8]); b_m8 = k.buf("m8")
        nbb = T("nbb", [128, 16], BF16); b_nbb = k.buf("nbb")
        nbT = T("nbT", [16, 128], BF16); b_nbT = k.buf("nbT")
        PT = [T(f"PT{i}", [128, 4, 128], BF16) for i in range(2)]; b_PT = k.bufs("PT", 2)
        rcp = T("rcp", [128, 1]); b_rcp = k.buf("rcp")
        ksum0 = T("ksum0", [128, 1]); b_ks0 = k.buf("ksum0")

        k.dma(sp, wa2s[:], wa2.ap(), writes=[b_wa2])
        k.dma(sp, vbc[:], vecbc.ap(), writes=[b_vbc])
        k.dma(sp, posi[:], pos_t.ap(), writes=[b_posi])
        wsrc = w_in_c.ap().rearrange("(k p) c -> p k c", p=128)
        for hh in range(4):
            k.dma(act, Wst[:, hh * 4:(hh + 1) * 4, :], wsrc[:, hh * 4:(hh + 1) * 4, :], writes=[b_Wst], cont=(hh > 0))
        TWO_PI = float(2 * np.pi)
        posf = T("posf", [128, 64]); b_posf = k.buf("posf")
        k.op(dve, lambda e: e.tensor_copy(posf[:], posi[:]), reads=[b_posi], writes=[b_posf])
        k.op(dve, lambda e: e.tensor_tensor(ang[:], posf[:].unsqueeze(2).to_broadcast([128, 64, 16]),
                                            invf.unsqueeze(1).to_broadcast([128, 64, 16]), ALU.mult),
             reads=[b_posf, b_cf], writes=[b_ang])
        angi = T("angi", [128, 64, 16], I32); b_angi = k.buf("angi")
        angn = T("angn", [128, 64, 16]); b_angn = k.buf("angn")
        PI = float(np.pi)

        def red_sin(dst, shift):
            k.op(dve, lambda e: e.tensor_scalar(dst[:], ang[:], shift, None, ALU.add), reads=[b_ang], writes=[b_cs])
            k.op(dve, lambda e: e.tensor_scalar(angn[:], dst[:], 1.0 / TWO_PI, None, ALU.mult), reads=[b_cs], writes=[b_angn])
            k.op(dve, lambda e: e.tensor_copy(angi[:], angn[:]), reads=[b_angn], writes=[b_angi])
            k.op(dve, lambda e: e.tensor_copy(angn[:], angi[:]), reads=[b_angi], writes=[b_angn])
            k.op(dve, lambda e: e.scalar_tensor_tensor(out=dst[:], in0=angn[:], scalar=-TWO_PI, in1=dst[:], op0=ALU.mult, op1=ALU.add),
                 reads=[b_angn, b_cs], writes=[b_cs])
            k.op(dve, lambda e: e.tensor_scalar(angn[:], dst[:], PI, -TWO_PI, ALU.is_ge, ALU.mult), reads=[b_cs], writes=[b_angn])
            k.op(dve, lambda e: e.tensor_tensor(dst[:], dst[:], angn[:], ALU.add), reads=[b_cs, b_angn], writes=[b_cs])
            k.op(dve, lambda e: e.tensor_scalar(angn[:], dst[:], -PI, TWO_PI, ALU.is_lt, ALU.mult), reads=[b_cs], writes=[b_angn])
            k.op(dve, lambda e: e.tensor_tensor(dst[:], dst[:], angn[:], ALU.add), reads=[b_cs, b_angn], writes=[b_cs])
            k.op(act, lambda e: e.activation(out=dst[:], in_=dst[:], func=AF.Sin), reads=[b_cs], writes=[b_cs])
        red_sin(sinT, 0.0)
        red_sin(cosT, PI / 2)
        k.op(pool, lambda e: e.memset(gaA[:], 1.0), writes=[b_gaA])
        k.op(pool, lambda e: e.memset(Vt[:], 1.0), writes=b_V)

        xsrc = x_all.ap().rearrange("(n p) d -> n p d", p=128)
        TRB = [PS[0], PS[1]]; b_TR = [PB_[0], PB_[1]]
        BA, BBk = PS[2], PS[3]; b_BA, b_BB = PB_[2], PB_[3]
        G1, G2 = PS[4], PS[5]; b_G1, b_G2 = PB_[4], PB_[5]
        MS, MO = PS[6], PS[7]; b_MS, b_MO = PB_[6], PB_[7]
        G1bf = G1[:, 384:512].bitcast(BF16)
        MObf1 = MO[:, 192:256].bitcast(BF16)
        MObf2 = MO[:, 256:384].bitcast(BF16)
        MObf3 = MO[:, 384:512].bitcast(BF16)

        def prep_weights(b):
            for kk in range(KC):
                eng = act if kk % 2 == 0 else dve
                if eng is act:
                    k.op(act, lambda e, kk=kk: e.activation(out=Wb[:, kk, :], in_=Wst[:, kk, :], func=AF.Copy, scale=A1[:, kk, b:b + 1]),
                         reads=[b_Wst, b_A1], writes=[b_Wb])
                else:
                    k.op(dve, lambda e, kk=kk: e.tensor_scalar(Wb[:, kk, :], Wst[:, kk, :], A1[:, kk, b:b + 1], None, ALU.mult),
                         reads=[b_Wst, b_A1], writes=[b_Wb])
            for (c0, c1) in ((0, 400), (400, 784)):
                for kk in range(KC):
                    k.op(pe, lambda e, kk=kk, c0=c0, c1=c1: e.matmul(BA[0:1, 0:c1 - c0], lhsT=B1p[:, kk, b:b + 1], rhs=Wb[:, kk, c0:c1],
                                                                      start=(kk == 0), stop=(kk == KC - 1)),
                         reads=[b_B1p, b_Wb], writes=[b_BA])
                k.op(act, lambda e, c0=c0, c1=c1: e.activation(out=brow[0:1, c0:c1], in_=BA[0:1, 0:c1 - c0], func=AF.Copy),
                     reads=[b_BA], writes=[b_brow])

        def stageA(g):
            s = g % NX
            k.dma(sp, xs[s][:], xsrc[g], writes=[b_xs[s]])
            k.op(pool, lambda e: e.memset(st[s][:, 0:1], 0.0), writes=[b_st[s]])
            k.op(act, lambda e: e.activation(out=hb[g % 2][:], in_=xs[s][:], func=AF.Square, accum_out=st[s][:, 0:1]),
                 reads=[b_xs[s]], writes=[b_st[s], b_hb[g % 2]])
            k.op(act, lambda e: e.activation(out=st[s][:, 1:2], in_=st[s][:, 0:1], func=AF.Ln, scale=1.0 / D, bias=epsc),
                 reads=[b_st[s]], writes=[b_st[s]])
            k.op(act, lambda e: e.activation(out=st[s][:, 2:3], in_=st[s][:, 1:2], func=AF.Exp, scale=-0.5),
                 reads=[b_st[s]], writes=[b_st[s]])
            h = g % 2
            k.op(act, lambda e: e.activation(out=hb[h][:], in_=xs[s][:], func=AF.Copy, scale=st[s][:, 2:3]),
                 reads=[b_xs[s], b_st[s]], writes=[b_hb[h]])

        def stageB(g):
            h = g % 2
            for half in range(2):
                tb = TRB[half][:, :].bitcast(BF16)
                for j in range(8):
                    kk = half * 8 + j
                    k.op(pe, lambda e, kk=kk, j=j, tb=tb: e.transpose(tb[:, j * 128:(j + 1) * 128], hb[h][:, kk * 128:(kk + 1) * 128], identb),
                         reads=[b_hb[h], b_cb], writes=[b_TR[half]])
                dst = hT[h][:, half * 8:(half + 1) * 8, :]
                if half == 0:
                    k.op(dve, lambda e, tb=tb, dst=dst: e.tensor_copy(dst, tb.rearrange("p (k t) -> p k t", k=8)),
                         reads=[b_TR[half]], writes=[b_hT[h]])
                else:
                    k.op(act, lambda e, tb=tb, dst=dst: e.activation(out=dst, in_=tb.rearrange("p (k t) -> p k t", k=8), func=AF.Copy),
                         reads=[b_TR[half]], writes=[b_hT[h]])
            for (bank, bb, c0, c1) in ((BA, b_BA, 0, 400), (BBk, b_BB, 400, 784)):
                for kk in range(KC):
                    k.op(pe, lambda e, kk=kk, bank=bank, c0=c0, c1=c1: e.matmul(bank[:, 0:c1 - c0], lhsT=hT[h][:, kk, :], rhs=Wb[:, kk, c0:c1],
                                                                                 start=(kk == 0), stop=False),
                         reads=[b_hT[h], b_Wb], writes=[bb])
                k.op(pe, lambda e, bank=bank, c0=c0, c1=c1: e.matmul(bank[:, 0:c1 - c0], lhsT=onesb[0:1, :], rhs=brow[0:1, c0:c1],
                                                                      start=False, stop=True),
                     reads=[b_cb, b_brow], writes=[bb])

        def stageC(g):
            b, tt = divmod(g, NT)
            if g == DBG_G:
                ddump(0, BA[:, 0:400], [b_BA])
                ddump(400, BBk[:, 0:384], [b_BB])
            k.op(act, lambda e: e.activation(out=gas[:], in_=BA[:, 128:144], func=AF.Copy), reads=[b_BA], writes=[b_gas])
            k.op(pe, lambda e: e.transpose(G1[0:16, 192:320], gas[:], ident), reads=[b_gas, b_cf], writes=[b_G1])
            k.op(dve, lambda e: e.tensor_copy(gaA[0:16, :], G1[0:16, 192:320]), reads=[b_G1], writes=[b_gaA])
            k.op(pe, lambda e: e.matmul(G1[:, 0:64], lhsT=gaA[:], rhs=wa2s[:], start=True, stop=True),
                 reads=[b_gaA, b_wa2], writes=[b_G1])
            k.op(act, lambda e: e.activation(out=l1[:], in_=G1[:, 0:64], func=AF.Exp, scale=-1.0), reads=[b_G1], writes=[b_l1])
            k.op(act, lambda e: e.activation(out=l1[:], in_=l1[:], func=AF.Ln, bias=1.0), reads=[b_l1], writes=[b_l1])
            k.op(pe, lambda e: e.matmul(G1[:, 64:128], lhsT=triUs, rhs=l1[:], start=True, stop=True), reads=[b_cf, b_l1], writes=[b_G1])
            k.op(pe, lambda e: e.matmul(G1[:, 128:192], lhsT=triLs, rhs=l1[:], start=True, stop=True), reads=[b_cf, b_l1], writes=[b_G1])
            k.op(pe, lambda e: e.matmul(G1[0:64, 320:321], lhsT=l1[:], rhs=negs, start=True, stop=True), reads=[b_cf, b_l1], writes=[b_G1])
            k.op(act, lambda e: e.activation(out=Eb[:, 0:128], in_=G1[:, 64:192], func=AF.Exp), reads=[b_G1], writes=[b_Eb])
            k.op(act, lambda e: e.activation(out=Eb[:, 128:192], in_=G1[:, 64:128], func=AF.Exp, scale=-1.0), reads=[b_G1], writes=[b_Eb])
            k.op(act, lambda e: e.activation(out=dec[:], in_=G1[0:64, 320:321], func=AF.Exp), reads=[b_G1], writes=[b_dec])
            k.op(dve, lambda e: e.scalar_tensor_tensor(out=qkd[:, 0:64], in0=BA[:, 0:64], scalar=0.125, in1=Eb[:, 0:64],
                                                       op0=ALU.mult, op1=ALU.mult), reads=[b_BA, b_Eb], writes=[b_qkd])
            k.op(dve, lambda e: e.tensor_tensor(qkd[:, 64:128], BA[:, 64:128], Eb[:, 128:192], ALU.mult), reads=[b_BA, b_Eb], writes=[b_qkd])
            k.op(dve, lambda e: e.tensor_tensor(qkd[:, 128:192], BA[:, 64:128], Eb[:, 64:128], ALU.mult), reads=[b_BA, b_Eb], writes=[b_qkd])
            k.op(act, lambda e: e.activation(out=vbf[:], in_=BA[:, 144:272], func=AF.Copy), reads=[b_BA], writes=[b_vbf])
            k.op(act, lambda e: e.activation(out=sg[:], in_=BA[:, 272:400], func=AF.Silu), reads=[b_BA], writes=[b_sg])
            k.op(pe, lambda e: e.transpose(G1bf[0:64, 0:128], qkd[:, 0:64], identb), reads=[b_qkd, b_cb], writes=[b_G1])
            k.op(pe, lambda e: e.transpose(G1bf[0:64, 128:256], qkd[:, 64:128], identb), reads=[b_qkd, b_cb], writes=[b_G1])
            k.op(dve, lambda e: e.tensor_copy(qkT[:], G1bf[0:64, :]), reads=[b_G1], writes=[b_qkT])
            k.op(pe, lambda e: e.matmul(G2[:, 0:128], lhsT=qkT[:, 128:256], rhs=qkT[:, 0:128], start=True, stop=True),
                 reads=[b_qkT], writes=[b_G2])
            k.op(dve, lambda e: e.tensor_tensor(atT[:], G2[:, 0:128], triU, ALU.mult), reads=[b_G2, b_cf], writes=[b_atT])
            si = g % 2
            k.op(pe, lambda e: e.matmul(G2[:, 128:256], lhsT=atT[:], rhs=vbf[:], start=True, stop=False), reads=[b_atT, b_vbf], writes=[b_G2])
            k.op(pe, lambda e: e.matmul(G2[:, 128:256], lhsT=qkT[:, 0:128], rhs=Sb[si][:], start=False, stop=True),
                 reads=[b_qkT, b_Sb[si]], writes=[b_G2])
            k.op(pe, lambda e: e.matmul(G2[0:64, 256:384], lhsT=qkd[:, 128:192], rhs=vbf[:], start=True, stop=True),
                 reads=[b_qkd, b_vbf], writes=[b_G2])
            k.op(dve, lambda e: e.scalar_tensor_tensor(out=Sf[:], in0=Sf[:], scalar=dec[:], in1=G2[0:64, 256:384],
                                                       op0=ALU.mult, op1=ALU.add), reads=[b_Sf, b_dec, b_G2], writes=[b_Sf])
            k.op(dve, lambda e: e.tensor_copy(Sb[1 - si][:], Sf[:]), reads=[b_Sf], writes=[b_Sb[1 - si]])
            k.op(pool, lambda e: e.memset(st[0][:, 4:5], 0.0), writes=[b_rcp])
            k.op(act, lambda e: e.activation(out=otmp[:], in_=G2[:, 128:256], func=AF.Square, accum_out=st[0][:, 4:5]),
                 reads=[b_G2, b_rcp], writes=[b_otmp, b_rcp])
            k.op(act, lambda e: e.activation(out=st[0][:, 5:6], in_=st[0][:, 4:5], func=AF.Ln, scale=1.0 / 128, bias=epsc), reads=[b_rcp], writes=[b_rcp])
            k.op(act, lambda e: e.activation(out=st[0][:, 5:6], in_=st[0][:, 5:6], func=AF.Exp, scale=-0.5), reads=[b_rcp], writes=[b_rcp])
            k.op(dve, lambda e: e.scalar_tensor_tensor(out=otmp[:], in0=G2[:, 128:256], scalar=st[0][:, 5:6], in1=vbc[:, 0:128],
                                                       op0=ALU.mult, op1=ALU.mult), reads=[b_G2, b_rcp, b_vbc, b_otmp], writes=[b_otmp])
            k.op(dve, lambda e: e.tensor_tensor(ocat[:, 0:128], otmp[:], sg[:], ALU.mult), reads=[b_otmp, b_sg], writes=[b_ocat])

            if CSUB < 2:
                return
            if g == DBG_G:
                ddump(800, l1[:], [b_l1])
                ddump(864, Eb[:], [b_Eb])
                ddump(1056, G2[:, 0:128], [b_G2])
                ddump(1184, G2[:, 128:256], [b_G2])
                ddump(1312, sg[:], [b_sg])
                ddump(1440, otmp[:], [b_otmp])
                ddump(1568, Sf[:], [b_Sf], p=64)
            nbk = tt // 2
            gl = b * NT + tt
            k.op(pool, lambda e: e.memset(st[1][:, 4:6], 0.0), writes=[b_m8])
            k.op(act, lambda e: e.activation(out=qkn[:, 0, :], in_=BBk[:, 0:128], func=AF.Square, accum_out=st[1][:, 4:5]),
                 reads=[b_BB, b_m8], writes=[b_qkn, b_m8])
            k.op(act, lambda e: e.activation(out=qkn[:, 1, :], in_=BBk[:, 256:384], func=AF.Square, accum_out=st[1][:, 5:6]),
                 reads=[b_BB, b_m8], writes=[b_qkn, b_m8])
            k.op(act, lambda e: e.activation(out=st[1][:, 6:8], in_=st[1][:, 4:6], func=AF.Ln, scale=1.0 / 128, bias=epsc), reads=[b_m8], writes=[b_m8])
            k.op(act, lambda e: e.activation(out=st[1][:, 6:8], in_=st[1][:, 6:8], func=AF.Exp, scale=-0.5), reads=[b_m8], writes=[b_m8])
            k.op(dve, lambda e: e.scalar_tensor_tensor(out=qkn[:, 0, :], in0=BBk[:, 0:128], scalar=st[1][:, 6:7], in1=vbc[:, 256:384],
                                                       op0=ALU.mult, op1=ALU.mult), reads=[b_BB, b_m8, b_vbc, b_qkn], writes=[b_qkn])
            k.op(dve, lambda e: e.scalar_tensor_tensor(out=qkn[:, 1, :], in0=BBk[:, 256:384], scalar=st[1][:, 7:8], in1=vbc[:, 128:256],
                                                       op0=ALU.mult, op1=ALU.mult), reads=[b_BB, b_m8, b_vbc, b_qkn], writes=[b_qkn])
            x1v = qkn[:, :, 0:16]
            x2v = qkn[:, :, 16:32]
            cs = cosT[:, gl, :].unsqueeze(1).to_broadcast([128, 2, 16])
            sn = sinT[:, gl, :].unsqueeze(1).to_broadcast([128, 2, 16])
            k.op(dve, lambda e: e.tensor_tensor(rt[:, 0], x1v, cs, ALU.mult), reads=[b_qkn, b_cs], writes=[b_rt])
            k.op(dve, lambda e: e.tensor_tensor(rt[:, 1], x2v, sn, ALU.mult), reads=[b_qkn, b_cs], writes=[b_rt])
            k.op(dve, lambda e: e.tensor_tensor(rt[:, 2], x2v, cs, ALU.mult), reads=[b_qkn, b_cs], writes=[b_rt])
            k.op(dve, lambda e: e.tensor_tensor(rt[:, 3], x1v, sn, ALU.mult), reads=[b_qkn, b_cs], writes=[b_rt])
            k.op(act, lambda e: e.activation(out=qkb[:, :, 32:128], in_=qkn[:, :, 32:128], func=AF.Copy), reads=[b_qkn], writes=[b_qkb])
            k.op(dve, lambda e: e.tensor_tensor(qkb[:, :, 0:16], rt[:, 0], rt[:, 1], ALU.subtract), reads=[b_rt], writes=[b_qkb])
            k.op(dve, lambda e: e.tensor_tensor(qkb[:, :, 16:32], rt[:, 2], rt[:, 3], ALU.add), reads=[b_rt], writes=[b_qkb])
            k.op(act, lambda e: e.activation(out=Vt[:, tt, 0:128], in_=BBk[:, 128:256], func=AF.Copy), reads=[b_BB], writes=[b_V[tt]])
            k.op(pe, lambda e: e.transpose(MObf2[:, 0:128], qkb[:, 0, :], identb), reads=[b_qkb, b_cb], writes=[b_MO])
            k.op(pe, lambda e: e.transpose(MObf2[:, 128:256], qkb[:, 1, :], identb), reads=[b_qkb, b_cb], writes=[b_MO])
            k.op(dve, lambda e: e.tensor_copy(KT[:, tt * 128:(tt + 1) * 128], MObf2[:, 0:128]), reads=[b_MO], writes=[b_KT[tt]])
            k.op(dve, lambda e: e.tensor_copy(qTc[:], MObf2[:, 128:256]), reads=[b_MO], writes=[b_qTc])
            if nbk > 0:
                k.op(pe, lambda e: e.matmul(MO[:, 136:152], lhsT=qTc[:], rhs=kmT[:], start=True, stop=True), reads=[b_qTc, b_km], writes=[b_MO])
                k.op(dve, lambda e: e.tensor_tensor(gsb[:], MO[:, 136:152], PBt[:, nbk * 16:(nbk + 1) * 16], ALU.add),
                     reads=[b_MO, b_cf], writes=[b_gsb])
                k.op(dve, lambda e: e.max(out=m8[:], in_=gsb[:]), reads=[b_gsb], writes=[b_m8])
                k.op(dve, lambda e: e.tensor_scalar(gsb[:], gsb[:], m8[:, 2:3], None, ALU.is_ge), reads=[b_gsb, b_m8], writes=[b_gsb])
                k.op(dve, lambda e: e.tensor_tensor(gsb[:], gsb[:], PMt[:, nbk * 16:(nbk + 1) * 16], ALU.mult), reads=[b_gsb, b_cf], writes=[b_gsb])
                k.op(dve, lambda e: e.tensor_scalar(nbb[:], gsb[:], -1.0, -NEG, ALU.add, ALU.mult), reads=[b_gsb], writes=[b_nbb])
                k.op(pe, lambda e: e.transpose(MObf1[0:16, :], nbb[:], identb), reads=[b_nbb, b_cb], writes=[b_MO])
                k.op(dve, lambda e: e.tensor_copy(nbT[:], MObf1[0:16, :]), reads=[b_MO], writes=[b_nbT])
            k.op(pe, lambda e: e.matmul(MO[:, 160:161], lhsT=qkb[:, 0, :], rhs=onesb[:, 0:1], start=True, stop=True),
                 reads=[b_qkb, b_cb], writes=[b_MO])
            if tt % 2 == 0:
                k.op(act, lambda e: e.activation(out=ksum0[:], in_=MO[:, 160:161], func=AF.Copy, scale=1.0 / 256),
                     reads=[b_MO], writes=[b_ks0])
            else:
                k.op(dve, lambda e: e.scalar_tensor_tensor(out=kmT[:, nbk:nbk + 1], in0=MO[:, 160:161], scalar=1.0 / 256, in1=ksum0[:],
                                                           op0=ALU.mult, op1=ALU.add), reads=[b_MO, b_ks0], writes=[b_km])
            if CSUB < 3:
                return
            SC = float(128 ** -0.5)
            ngrp = (tt + 4) // 4
            for gi in range(ngrp):
                kts = list(range(gi * 4, min(gi * 4 + 4, tt + 1)))
                pi = gi % 2
                for j, kt in enumerate(kts):
                    past = kt < 2 * nbk
                    k.op(pe, lambda e, j=j, kt=kt, past=past: e.matmul(MS[:, j * 128:(j + 1) * 128], lhsT=KT[:, kt * 128:(kt + 1) * 128], rhs=qTc[:],
                                                                        start=True, stop=not past),
                         reads=[b_KT[kt], b_qTc], writes=[b_MS])
                    if past:
                        k.op(pe, lambda e, j=j, kt=kt: e.matmul(MS[:, j * 128:(j + 1) * 128], lhsT=indb[0:16, kt // 2, :], rhs=nbT[:],
                                                                 start=False, stop=True),
                             reads=[b_cb, b_nbT], writes=[b_MS])
                n = len(kts)
                k.op(act, lambda e, n=n, pi=pi: e.activation(out=PT[pi][:, 0:n, :], in_=MS[:, 0:n * 128].rearrange("p (a q) -> p a q", a=n),
                                                             func=AF.Exp, scale=SC),
                     reads=[b_MS], writes=[b_PT[pi]])
                if kts[-1] == tt:
                    jl = n - 1
                    k.op(dve, lambda e, jl=jl, pi=pi: e.tensor_tensor(PT[pi][:, jl, :], PT[pi][:, jl, :], triUb, ALU.mult),
                         reads=[b_PT[pi], b_cb], writes=[b_PT[pi]])
                for j, kt in enumerate(kts):
                    k.op(pe, lambda e, j=j, kt=kt, pi=pi: e.matmul(MO[:, 0:129], lhsT=PT[pi][:, j, :], rhs=Vt[:, kt, 0:129],
                                                                    start=(kt == 0), stop=(kt == tt)),
                         reads=[b_PT[pi], b_V[kt]], writes=[b_MO])
            k.op(dve, lambda e: e.reciprocal(rcp[:], MO[:, 128:129]), reads=[b_MO], writes=[b_rcp])
            k.op(dve, lambda e: e.tensor_scalar(ocat[:, 128:256], MO[:, 0:128], rcp[:], None, ALU.mult), reads=[b_MO, b_rcp], writes=[b_ocat])
            if g == DBG_G:
                ddump(1700, qkn[:, 0, :], [b_qkn])
                ddump(1828, qkn[:, 1, :], [b_qkn])
                ddump(1956, MO[:, 0:129], [b_MO])
                ddump(2100, gsb[:], [b_gsb])
            oi = (g // 4) % 2
            for c in range(2):
                k.op(pe, lambda e, c=c: e.transpose(MObf3[:, c * 128:(c + 1) * 128], ocat[:, c * 128:(c + 1) * 128], identb),
                     reads=[b_ocat, b_cb], writes=[b_MO])
            k.op(dve, lambda e: e.tensor_copy(oTs[oi][:, :, (g % 4) * 128:(g % 4 + 1) * 128], MObf3.rearrange("p (c t) -> p c t", c=2)),
                 reads=[b_MO], writes=[b_oTs[oi]])
            if g % 4 == 3:
                t0 = (g - 3) * 128
                k.dma(sp, ib.ap().rearrange("(c p) t -> p c t", p=128)[:, :, t0:t0 + 512], oTs[oi][:], reads=[b_oTs[oi]])

        import os as _os
        NG = int(_os.environ.get('DEV_NG', 2 * NT))
        if _os.environ.get('DEV_SKIP01'):
            NG = 0
        SUB = int(_os.environ.get('DEV_SUB', 3))
        DBG_G = int(_os.environ.get('DEV_DBGG', 0))
        CSUB = int(_os.environ.get('DEV_CSUB', 3))
        for step in range(NG + 2):
            if step < NG and SUB >= 1:
                stageA(step)
            gC = step - 2
            if 0 <= gC < NG and SUB >= 3:
                if gC % NT == 0:
                    k.op(dve, lambda e: e.memset(Sf[:], 0.0), writes=[b_Sf])
                    k.op(dve, lambda e: e.memset(kmT[:], 0.0), writes=[b_km])
                    k.op(dve, lambda e, si=gC % 2: e.memset(Sb[si][:], 0.0), writes=[b_Sb[gC % 2]])
                stageC(gC)
            gB = step - 1
            if 0 <= gB < NG and SUB >= 2:
                if gB % NT == 0:
                    prep_weights(gB // NT)
                stageB(gB)
        k.barrier()
        if dbg:
            k.dma(sp, d_ib.ap(), ib.ap(), sembuf=k.buf("dib"))
            if stage == 1:
                k.dma(sp, d_dbg.ap(), dbgt[:], reads=[b_dbgt])
            k.barrier()
    if stage <= 1:
        k.barrier()
        return k

    h2T = sb("h2T", [128, KC, 1024], BF16); b_h2T = k.bufs("h2T", 8)
    g2bc = sb("g2bc", [128, D]); b_g2bc = k.buf("g2bc")
    b_ag = k.buf("ag")
    k.dma(pool, None, None, sembuf=b_ag, inc=1, writes=[b_ag],
          fn=lambda e: e.collective_compute("AllGather", ALU.bypass, replica_groups=[list(range(8))], ins=[ib.ap()], outs=[ob.ap()]))
    with contextlib.ExitStack() as es:
        def T(name, shape, dt=F32):
            return es.enter_context(nc.sbuf_tensor(name, list(shape), dt))
        xo = T("xo", [128, 8, D]); b_xo = k.bufs("xo", 8)
        mixT = T("mixT", [128, KC, 1024], BF16); b_mixT = k.buf("mixT")
        Wo = [T(f"Wo{i}", [128, KC, 512], BF16) for i in range(2)]; b_Wo = k.bufs("Wo", 2)
        bc3f = T("bc3f", [128, D]); b_bc3f = k.buf("bc3f")
        bc3 = T("bc3", [128, 3, D], BF16); b_bc3 = k.buf("bc3")
        h2b = T("h2b", [128, D], BF16); b_h2b = k.buf("h2b")
        st2 = T("stn2", [128, 8]); b_st2 = k.buf("stn2")
        for vi, kind in enumerate(("gate1", "A2", "B2")):
            build_bcast([(kind, bc3f, b_bc3f)])
            k.op(dve, lambda e, vi=vi: e.tensor_copy(bc3[:, vi, :], bc3f[:]), reads=[b_bc3f], writes=[b_bc3])
        build_bcast([("gate2", g2bc, b_g2bc)])
        pid = nc.gpsimd.partition_id()
        msrc = ob.ap()[:, bass.ts(pid, 1024)].rearrange("(k p) t -> p k t", p=128)
        k.dma(pool, mixT[:], msrc, reads=[b_ag], writes=[b_mixT])
        xsrc2 = x_own.ap().rearrange("(n p) d -> p n d", p=128)
        b_xol = k.buf("xol")
        for t in range(8):
            k.dma(sp, xo[:, t, :], xsrc2[:, t, :], writes=[b_xo[t]], sembuf=b_xol)
        for t in range(8):
            b_xo[t].writer = ("D", b_xol.dsem, b_xol.dcnt)
        wosrc = w_out_p.ap().rearrange("(k p) c -> p k c", p=128)
        for cg in range(4):
            w_ = cg % 2
            k.dma(pool, Wo[w_][:], wosrc[:, :, cg * 512:(cg + 1) * 512], writes=[b_Wo[w_]])
            for t in range(8):
                bank = t % 2
                for kk in range(KC):
                    k.op(pe, lambda e, kk=kk, t=t, w_=w_, bank=bank: e.matmul(PS[bank][:], lhsT=mixT[:, kk, t * 128:(t + 1) * 128], rhs=Wo[w_][:, kk, :],
                                                                               start=(kk == 0), stop=(kk == KC - 1)),
                         reads=[b_mixT, b_Wo[w_]], writes=[PB_[bank]])
                k.op(dve, lambda e, t=t, cg=cg, bank=bank: e.tensor_tensor(h2b[:, 0:512], PS[bank][:], bc3[:, 0, cg * 512:(cg + 1) * 512], ALU.mult),
                     reads=[PB_[bank], b_bc3], writes=[b_h2b])
                k.op(dve, lambda e, t=t, cg=cg: e.tensor_tensor(xo[:, t, cg * 512:(cg + 1) * 512], xo[:, t, cg * 512:(cg + 1) * 512], h2b[:, 0:512], ALU.add),
                     reads=[b_h2b, b_xo[t]], writes=[b_xo[t]])
        x1dst = x1d.ap().rearrange("(n p) d -> p n d", p=128)
        b_x1d = k.buf("x1d")
        b_dx1 = k.buf("dx1")
        for t in range(8):
            k.dma(sp, x1dst[:, t, :], xo[:, t, :], reads=[b_xo[t]], writes=[b_x1d], sembuf=b_x1d)
            if dbg:
                k.dma(sp, d_x1.ap().rearrange("(n p) d -> p n d", p=128)[:, t, :], xo[:, t, :], reads=[b_xo[t]], sembuf=b_dx1)
            k.op(pool, lambda e: e.memset(st2[:, 0:1], 0.0), writes=[b_st2])
            k.op(act, lambda e, t=t: e.activation(out=h2b[:], in_=xo[:, t, :], func=AF.Square, accum_out=st2[:, 0:1]),
                 reads=[b_xo[t]], writes=[b_st2, b_h2b])
            k.op(act, lambda e: e.activation(out=st2[:, 1:2], in_=st2[:, 0:1], func=AF.Ln, scale=1.0 / D, bias=epsc), reads=[b_st2], writes=[b_st2])
            k.op(act, lambda e: e.activation(out=st2[:, 2:3], in_=st2[:, 1:2], func=AF.Exp, scale=-0.5), reads=[b_st2], writes=[b_st2])
            k.op(dve, lambda e, t=t: e.scalar_tensor_tensor(out=h2b[:], in0=xo[:, t, :], scalar=st2[:, 2:3], in1=bc3[:, 1, :], op0=ALU.mult, op1=ALU.mult),
                 reads=[b_xo[t], b_st2, b_bc3], writes=[b_h2b])
            k.op(dve, lambda e: e.tensor_tensor(h2b[:], h2b[:], bc3[:, 2, :], ALU.add), reads=[b_h2b, b_bc3], writes=[b_h2b])
            for half in range(2):
                bank = 2 + half
                tb = PS[bank][:, :].bitcast(BF16)
                for j in range(8):
                    kk = half * 8 + j
                    k.op(pe, lambda e, kk=kk, j=j, tb=tb: e.transpose(tb[:, j * 128:(j + 1) * 128], h2b[:, kk * 128:(kk + 1) * 128], identb),
                         reads=[b_h2b, b_cb], writes=[PB_[bank]])
                dst = h2T[:, half * 8:(half + 1) * 8, t * 128:(t + 1) * 128]
                k.op(act, lambda e, tb=tb, dst=dst: e.activation(out=dst, in_=tb.rearrange("p (k t) -> p k t", k=8), func=AF.Copy),
                     reads=[PB_[bank]], writes=[b_h2T[t]])
        k.barrier()
    if stage <= 2:
        k.barrier()
        return k

    skTb = sb("skTb", [128, 16, 128], BF16); b_skTb = k.buf("skTb")
    k.dma(pool, skTb[:], skT.ap().rearrange("p (a m) -> p a m", a=16), writes=[b_skTb])
    wqsrc = w_q.ap().rearrange("(k p) c -> p k c", p=128)
    usrc = uT.ap().rearrange("(k p) e -> p k e", p=128)
    ydst = y.ap().rearrange("(n p) d -> p n d", p=128)
    x1src = x1d.ap().rearrange("(n p) d -> p n d", p=128)
    b_Wq = k.bufs("Wq", 2)
    b_us = k.bufs("us", 2)
    b_vs = k.bufs("vs", 8)
    b_x1t = k.buf("x1t")
    b_yst = k.buf("yst")
    for tg in range(2):
        with contextlib.ExitStack() as esg:
            def TG(name, shape, dt=F32):
                return esg.enter_context(nc.sbuf_tensor(f"{name}_{tg}", list(shape), dt))
            EA = TG("EA", [128, 4, 8, 128], F32); b_EA = k.bufs("EA", 4)
            EB = TG("EB", [128, 4, 8, 128], BF16); b_EB = k.bufs("EB", 4)
            thr = TG("thr", [128, 4, 8]); b_thr = k.bufs("thr", 4)
            with contextlib.ExitStack() as es:
                def T(name, shape, dt=F32):
                    return es.enter_context(nc.sbuf_tensor(f"{name}_{tg}", list(shape), dt))
                qT = T("qT", [128, 16, 512], BF16); b_qT = k.bufs("qT", 16)
                Wq = [T(f"Wq{i}", [128, KC, 512], BF16) for i in range(2)]
                ssb = T("ssb", [128, 16, 128]); b_ssb = k.buf("ssb")
                m16 = T("m16", [128, 16, 16]); b_m16 = k.buf("m16")
                tmpk = T("tmpk", [128, 128]); b_tmpk = k.buf("tmpk")
                cand = T("cand", [128, 16, 16]); b_cand = k.buf("cand")
                tc1 = T("tc1", [128, 256]); b_tc1 = k.buf("tc1")
                tc2 = T("tc2", [128, 256]); b_tc2 = k.buf("tc2")
                t24 = T("t24", [128, 8, 24]); b_t24 = k.buf("t24")
                e16 = T("e16", [128, 8, 16]); b_e16 = k.buf("e16")
                zz = T("zz", [128, 4, 8]); b_zz = k.buf("zz")
                for cg in range(4):
                    w_ = cg % 2
                    k.dma(pool, Wq[w_][:], wqsrc[:, :, cg * 512:(cg + 1) * 512], writes=[b_Wq[w_]])
                    for hl in range(4):
                        hc = cg * 4 + hl
                        bank = hc % 2
                        for kk in range(KC):
                            k.op(pe, lambda e, kk=kk, hl=hl, w_=w_, bank=bank: e.matmul(
                                PS[bank][:], lhsT=Wq[w_][:, kk, hl * 128:(hl + 1) * 128], rhs=h2T[:, kk, tg * 512:(tg + 1) * 512],
                                start=(kk == 0), stop=(kk == KC - 1)), reads=[b_Wq[w_]] + b_h2T[tg * 4:(tg + 1) * 4], writes=[PB_[bank]])
                        k.op(act, lambda e, hc=hc, bank=bank: e.activation(out=qT[:, hc, :], in_=PS[bank][:], func=AF.Copy),
                             reads=[PB_[bank]], writes=[b_qT[hc]])
                for tt in range(4):
                    for hc in range(16):
                        bank = 2 + hc // 4
                        k.op(pe, lambda e, hc=hc, bank=bank, tt=tt: e.matmul(PS[bank][:, (hc % 4) * 128:(hc % 4 + 1) * 128],
                                                                          lhsT=qT[:, hc, tt * 128:(tt + 1) * 128], rhs=skTb[:, hc, :],
                                                                          start=True, stop=True),
                             reads=[b_qT[hc], b_skTb], writes=[PB_[bank]])
                    for q4 in range(4):
                        k.op(act, lambda e, q4=q4: e.activation(out=ssb[:, q4 * 4:(q4 + 1) * 4, :],
                                                                 in_=PS[2 + q4][:].rearrange("p (a m) -> p a m", a=4), func=AF.Copy),
                             reads=[PB_[2 + q4]], writes=[b_ssb])
                    for hc in range(16):
                        k.op(dve, lambda e, hc=hc: e.max(out=m16[:, hc, 0:8], in_=ssb[:, hc, :]), reads=[b_ssb], writes=[b_m16])
                        k.op(dve, lambda e, hc=hc: e.match_replace(out=tmpk[:], in_to_replace=m16[:, hc, 0:8], in_values=ssb[:, hc, :], imm_value=-1e30),
                             reads=[b_ssb, b_m16], writes=[b_tmpk])
                        k.op(dve, lambda e, hc=hc: e.max(out=m16[:, hc, 8:16], in_=tmpk[:]), reads=[b_tmpk], writes=[b_m16])
                    for h in range(8):
                        k.op(dve, lambda e, h=h: e.tensor_tensor(cand[:], m16[:, 2 * h, :].unsqueeze(2).to_broadcast([128, 16, 16]),
                                                                 m16[:, 2 * h + 1, :].unsqueeze(1).to_broadcast([128, 16, 16]), ALU.add),
                             reads=[b_m16], writes=[b_cand])
                        cf_ = cand[:].rearrange("p a b -> p (a b)")
                        k.op(dve, lambda e, h=h: e.max(out=t24[:, h, 0:8], in_=cf_), reads=[b_cand], writes=[b_t24])
                        k.op(dve, lambda e, h=h: e.match_replace(out=tc1[:], in_to_replace=t24[:, h, 0:8], in_values=cf_, imm_value=-1e30),
                             reads=[b_cand, b_t24], writes=[b_tc1])
                        k.op(dve, lambda e, h=h: e.max(out=t24[:, h, 8:16], in_=tc1[:]), reads=[b_tc1], writes=[b_t24])
                        k.op(dve, lambda e, h=h: e.match_replace(out=tc2[:], in_to_replace=t24[:, h, 8:16], in_values=tc1[:], imm_value=-1e30),
                             reads=[b_tc1, b_t24], writes=[b_tc2])
                        k.op(dve, lambda e, h=h: e.max(out=t24[:, h, 16:24], in_=tc2[:]), reads=[b_tc2], writes=[b_t24])
                    mx = t24[:, :, 0:1]
                    k.op(dve, lambda e: e.tensor_tensor(e16[:], t24[:, :, 0:16], mx.to_broadcast([128, 8, 16]), ALU.subtract), reads=[b_t24], writes=[b_e16])
                    k.op(act, lambda e: e.activation(out=e16[:], in_=e16[:], func=AF.Exp), reads=[b_e16], writes=[b_e16])
                    k.op(dve, lambda e: e.reduce_sum(out=zz[:, 0, :], in_=e16[:], axis=AX.X), reads=[b_e16], writes=[b_zz])
                    k.op(dve, lambda e: e.reciprocal(zz[:, 1, :], zz[:, 0, :]), reads=[b_zz], writes=[b_zz])
                    k.op(dve, lambda e: e.tensor_tensor(zz[:, 2, :], t24[:, :, 15], t24[:, :, 16], ALU.add), reads=[b_t24], writes=[b_zz])
                    k.op(dve, lambda e: e.scalar_tensor_tensor(out=zz[:, 2, :], in0=zz[:, 2, :], scalar=0.5, in1=t24[:, :, 0], op0=ALU.mult, op1=ALU.subtract),
                         reads=[b_zz, b_t24], writes=[b_zz])
                    k.op(act, lambda e: e.activation(out=zz[:, 2, :], in_=zz[:, 2, :], func=AF.Exp), reads=[b_zz], writes=[b_zz])
                    k.op(dve, lambda e, tt=tt: e.tensor_tensor(thr[:, tt, :], zz[:, 2, :], zz[:, 1, :], ALU.mult), reads=[b_zz], writes=[b_thr[tt]])
                    k.op(dve, lambda e: e.tensor_tensor(ssb[:], ssb[:], m16[:, :, 0:1].to_broadcast([128, 16, 128]), ALU.subtract),
                         reads=[b_ssb, b_m16], writes=[b_ssb])
                    k.op(act, lambda e: e.activation(out=ssb[:], in_=ssb[:], func=AF.Exp), reads=[b_ssb], writes=[b_ssb])
                    sv = ssb[:].rearrange("p (h c) m -> p h c m", c=2)
                    k.op(dve, lambda e, tt=tt, sv=sv: e.tensor_tensor(EA[:, tt], sv[:, :, 0, :], zz[:, 1, :].unsqueeze(2).to_broadcast([128, 8, 128]), ALU.mult),
                         reads=[b_ssb, b_zz], writes=[b_EA[tt]])
                    k.op(act, lambda e, tt=tt, sv=sv: e.activation(out=EB[:, tt], in_=sv[:, :, 1, :], func=AF.Copy), reads=[b_ssb], writes=[b_EB[tt]])
                k.barrier()
            with contextlib.ExitStack() as es:
                def T(name, shape, dt=F32):
                    return es.enter_context(nc.sbuf_tensor(f"{name}_{tg}", list(shape), dt))
                acc = T("acc", [128, 4, D]); b_acc = k.bufs("acc", 4)
                NU = 2
                us = [T(f"us{i}", [128, KC, 512], BF16) for i in range(NU)]
                NV = 8
                vs = [T(f"vs{i}", [128, D], BF16) for i in range(NV)]
                actT = T("actT", [128, 8, 512], BF16); b_actT = k.bufs("actT", 8)
                NY = 4
                yt = [T(f"yt{i}", [128, 8, 128], BF16) for i in range(NY)]; b_yt = k.bufs("yt", NY)
                NGB = 5
                Gt = [T(f"Gt{i}", [128, 8, 128], BF16) for i in range(NGB)]; b_Gt = k.bufs("Gt", NGB)
                WT = [T(f"WT{i}", [128, 8, 128], BF16) for i in range(2)]; b_WT = k.bufs("WT", 2)
                x1t = actT[:].rearrange("p a b -> p (a b)").bitcast(F32)
                for tt in range(4):
                    k.op(dve, lambda e, tt=tt: e.memset(acc[:, tt, :], 0.0), writes=[b_acc[tt]])
                ycnt = [0]
                gcnt = [0]
                GTB = [(2, 3), (4, 5)]
                OBK = (6, 7)

                def load_u(c):
                    for eh in range(2):
                        e0 = c * 1024 + eh * 512
                        k.dma(pool, us[eh][:], usrc[:, :, e0:e0 + 512], writes=[b_us[eh]])

                def load_v(c):
                    for e8 in range(8):
                        e0 = c * 1024 + e8 * 128
                        k.dma(pool, vs[e8][:], vv.ap()[e0:e0 + 128, :], writes=[b_vs[e8]])

                def a_phase(c, e8s=range(8)):
                    for e8 in e8s:
                        bank = e8 % 2
                        su = e8 // 4
                        for kk in range(KC):
                            k.op(pe, lambda e, kk=kk, e8=e8, su=su, bank=bank: e.matmul(
                                PS[bank][:], lhsT=us[su][:, kk, (e8 % 4) * 128:(e8 % 4 + 1) * 128], rhs=h2T[:, kk, tg * 512:(tg + 1) * 512],
                                start=(kk == 0), stop=(kk == KC - 1)), reads=[b_us[su]] + b_h2T[tg * 4:(tg + 1) * 4], writes=[PB_[bank]])
                        k.op(act, lambda e, e8=e8, bank=bank: e.activation(out=actT[:, e8, :], in_=PS[bank][:], func=AF.Gelu),
                             reads=[PB_[bank]], writes=[b_actT[e8]])

                def gen(u, hs=range(8)):
                    c, tt = divmod(u, 4)
                    i0 = c * 8
                    banks = GTB[u % 2]
                    for h in hs:
                        ys = ycnt[0] % NY; ycnt[0] += 1
                        gs = gcnt[0] % NGB; gcnt[0] += 1
                        if h in (2, 5, 7):
                            k.op(dve, lambda e, tt=tt, h=h, ys=ys: e.tensor_tensor(
                                yt[ys][:], EA[:, tt, h, i0:i0 + 8].unsqueeze(2).to_broadcast([128, 8, 128]),
                                EB[:, tt, h, :].unsqueeze(1).to_broadcast([128, 8, 128]), ALU.mult),
                                reads=[b_EA[tt], b_EB[tt]], writes=[b_yt[ys]])
                        else:
                            for il in range(8):
                                k.op(act, lambda e, tt=tt, h=h, ys=ys, il=il: e.activation(
                                    out=yt[ys][:, il, :], in_=EB[:, tt, h, :], func=AF.Copy, scale=EA[:, tt, h, i0 + il:i0 + il + 1]),
                                    reads=([b_EA[tt], b_EB[tt]] if il == 0 else []), writes=([b_yt[ys]] if il == 0 else []))
                            b_yt[ys].writer = ("E", "act", act.total - 1)
                        k.op(dve, lambda e, tt=tt, h=h, ys=ys, gs=gs: e.scalar_tensor_tensor(
                            out=Gt[gs][:], in0=yt[ys][:], scalar=thr[:, tt, h:h + 1], in1=yt[ys][:], op0=ALU.is_ge, op1=ALU.mult),
                            reads=[b_yt[ys], b_thr[tt]], writes=[b_Gt[gs]])
                        for e8 in range(8):
                            gb = banks[e8 // 4]
                            k.op(pe, lambda e, e8=e8, gs=gs, gb=gb, h=h: e.matmul(
                                PS[gb][:, (e8 % 4) * 128:(e8 % 4 + 1) * 128], lhsT=Gt[gs][:, e8, :], rhs=identb,
                                start=(h == 0 and e8 % 4 == 0), stop=(h == 7), skip_group_check=True),
                                reads=[b_Gt[gs], b_cb], writes=[PB_[gb]])

                def wt_op(u):
                    c, tt = divmod(u, 4)
                    ws = u % 2
                    banks = GTB[u % 2]
                    for eh in range(2):
                        gb = banks[eh]
                        k.op(dve, lambda e, ws=ws, gb=gb, eh=eh, tt=tt: e.tensor_tensor(
                            WT[ws][:, eh * 4:(eh + 1) * 4, :], PS[gb][:].rearrange("p (a t) -> p a t", a=4),
                            actT[:, eh * 4:(eh + 1) * 4, tt * 128:(tt + 1) * 128], ALU.mult),
                            reads=[PB_[gb]] + b_actT[eh * 4:(eh + 1) * 4], writes=[b_WT[ws]])

                def out_mm(u, rnd):
                    ws = u % 2
                    for dl in range(2):
                        dg_ = rnd * 2 + dl
                        for e8 in range(8):
                            k.op(pe, lambda e, dg_=dg_, dl=dl, e8=e8, ws=ws: e.matmul(
                                PS[OBK[dl]][:], lhsT=WT[ws][:, e8, :], rhs=vs[e8][:, dg_ * 512:(dg_ + 1) * 512],
                                start=(e8 == 0), stop=(e8 == 7)),
                                reads=[b_WT[ws], b_vs[e8]], writes=[PB_[OBK[dl]]])

                def acc_add(u, rnd):
                    c, tt = divmod(u, 4)
                    for dl in range(2):
                        dg_ = rnd * 2 + dl
                        k.op(dve, lambda e, dg_=dg_, dl=dl, tt=tt: e.tensor_tensor(acc[:, tt, dg_ * 512:(dg_ + 1) * 512], acc[:, tt, dg_ * 512:(dg_ + 1) * 512],
                                                                                 PS[OBK[dl]][:], ALU.add),
                             reads=[PB_[OBK[dl]], b_acc[tt]], writes=[b_acc[tt]])

                NUNIT = 64
                load_u(0)
                load_v(0)
                for u in range(NUNIT + 1):
                    c, tt = divmod(u, 4)
                    if u >= 1:
                        wt_op(u - 1)
                        out_mm(u - 1, 0)
                    if u < NUNIT and tt == 0:
                        for h in range(8):
                            gen(u, [h])
                            a_phase(c, [h])
                        if c + 1 < 16:
                            load_u(c + 1)
                    elif u < NUNIT:
                        gen(u)
                    if u >= 1:
                        acc_add(u - 1, 0)
                        out_mm(u - 1, 1)
                        acc_add(u - 1, 1)
                        if (u - 1) % 4 == 3 and (u - 1) // 4 + 1 < 16:
                            load_v((u - 1) // 4 + 1)
                for tt in range(4):
                    tglob = tg * 4 + tt
                    k.dma(sp, x1t, x1src[:, tglob, :], reads=[b_x1d], writes=[b_x1t] + b_actT)
                    k.op(dve, lambda e, tt=tt: e.tensor_tensor(acc[:, tt, :], acc[:, tt, :], g2bc[:], ALU.mult), reads=[b_acc[tt], b_g2bc], writes=[b_acc[tt]])
                    k.op(dve, lambda e, tt=tt: e.tensor_tensor(acc[:, tt, :], acc[:, tt, :], x1t, ALU.add), reads=[b_acc[tt], b_x1t], writes=[b_acc[tt]])
                    k.dma(sp, ydst[:, tglob, :], acc[:, tt, :], reads=[b_acc[tt]], sembuf=b_yst)
                k.barrier()
    k.barrier()
    return k


def _prep_inputs(inp):
    x = np.ascontiguousarray(np.asarray(inp["x"], np.float32)).reshape(NTOK, D)
    c = np.asarray(inp["c"], np.float32)
    pos = np.asarray(inp["positions"], np.int32)
    w_in = np.asarray(inp["w_in"], np.float32)
    offs = np.cumsum([0, 512, 512, 1024, 1024, 16, 1024, 1024, 1024])
    gq, gk, gv, gg, ga, mq, mk, mv = [w_in[:, offs[i]:offs[i + 1]] for i in range(8)]
    w_out = np.asarray(inp["w_out"], np.float32)
    perm = []
    for r in range(8):
        perm += list(range(r * 128, (r + 1) * 128)) + list(range(1024 + r * 128, 1024 + (r + 1) * 128))
    w_out_p = np.ascontiguousarray(w_out[perm])
    cs = _consts()
    b_ada_fm = np.ascontiguousarray(np.asarray(inp["b_ada"], np.float32).reshape(96, 128).T)
    g12 = np.ascontiguousarray(np.concatenate([np.asarray(inp["norm1_g"], np.float32).reshape(16, 128).T,
                                               np.asarray(inp["norm2_g"], np.float32).reshape(16, 128).T], 1))
    vecbc = np.ascontiguousarray(np.broadcast_to(np.concatenate(
        [np.asarray(inp["gla_norm_g"], np.float32), np.asarray(inp["moba_q_norm_g"], np.float32),
         np.asarray(inp["moba_k_norm_g"], np.float32)])[None, :], (128, 384)))
    pos_t = np.ascontiguousarray(pos.reshape(64, 128).T)
    skT = np.ascontiguousarray(np.asarray(inp["peer_sub_keys"], np.float32).reshape(16, 128, 128).transpose(2, 0, 1).reshape(128, 2048))
    uT = np.ascontiguousarray(np.asarray(inp["peer_u"], np.float32).T)
    vv = np.ascontiguousarray(np.asarray(inp["peer_v"], np.float32))
    w_ada = np.ascontiguousarray(np.asarray(inp["w_ada"], np.float32))
    w_q = np.ascontiguousarray(np.asarray(inp["peer_w_q"], np.float32))
    wa2_full = np.asarray(inp["gla_w_a2"], np.float32)
    ba = np.asarray(inp["gla_b_a"], np.float32)
    maps = []
    for r in range(8):
        b = r // 4
        hs = slice(r * 64, (r + 1) * 64)
        hv = slice(r * 128, (r + 1) * 128)
        w_in_c = np.ascontiguousarray(np.concatenate([gq[:, hs], gk[:, hs], ga, gv[:, hv], gg[:, hv], mk[:, hv], mv[:, hv], mq[:, hv]], 1))
        c3 = np.stack([c[0], c[1], c[b]], -1).reshape(16, 128, 3).transpose(1, 0, 2).reshape(128, 48)
        wa2 = np.concatenate([wa2_full[:, hs], ba[None, hs]], 0)
        maps.append({
            "x_all": x, "x_own": np.ascontiguousarray(x[r * 1024:(r + 1) * 1024]), "c3": np.ascontiguousarray(c3),
            "w_ada": w_ada, "b_ada_fm": b_ada_fm, "g12_fm": g12, "w_in_c": w_in_c, "wa2": np.ascontiguousarray(wa2),
            "vecbc": vecbc, "pos_t": pos_t, "w_out_p": w_out_p, "w_q": w_q, "skT": skT, "uT": uT, "vv": vv,
            "cf": cs["cf"], "cb": cs["cb"],
        })
    return maps


def kernel(**inputs):
    maps = _prep_inputs(inputs)
    nc = build(stage=9, dbg=False)
    res = run_bass_kernel_spmd(nc, maps, core_ids=list(range(8)))
    out = np.concatenate([np.asarray(r["y"], np.float32) for r in res.results], 0)
    return out.reshape(2, SEQ, D)
```
